# Optimizing a Trainium2 kernel written in Bass

```python
import jax
import jax.numpy as jnp
from jax import lax
import numpy as np

D_MODEL = 1024
BATCH = 4
SEQ = 8192
DEPTH = 1

N_META = 16
EPS = 1e-6

POOL_WINDOWS = (2, 4, 8, 16)
POOL_WIDTH = D_MODEL
POOL_GROUP = POOL_WIDTH // len(POOL_WINDOWS)

SSD_INNER = 2 * D_MODEL
SSD_HEAD_DIM = 64
SSD_HEADS = SSD_INNER // SSD_HEAD_DIM
SSD_GROUPS = 4
SSD_HPG = SSD_HEADS // SSD_GROUPS
SSD_STATE = 128
SSD_CONV = 4
SSD_CHUNK = 128
SSD_CONV_DIM = SSD_INNER + 2 * SSD_GROUPS * SSD_STATE
SSD_LEAD_PAD = SSD_CHUNK - N_META

N_EXPERT_GROUPS = 8
EXPERTS_PER_GROUP = 8
N_EXPERTS = N_EXPERT_GROUPS * EXPERTS_PER_GROUP
TOP_K = 2
D_EXPERT = 512
MOE_BLOCK = 256

IN_WIDTHS = (POOL_WIDTH, SSD_INNER, SSD_CONV_DIM, SSD_HEADS, D_MODEL, D_MODEL)
IN_SPLITS = tuple(int(s) for s in np.cumsum(IN_WIDTHS[:-1]))
D_IN_PROJ = sum(IN_WIDTHS)

kernel_name = 'hybrid_pool_ssd_hier_moe'


def rms_norm(x, g):
    xf = x.astype(jnp.float32)
    y = xf * lax.rsqrt(jnp.mean(xf * xf, axis=-1, keepdims=True) + EPS)
    return (y * g.astype(jnp.float32)).astype(x.dtype)


def pool_mixer(u, w_pool, pool_scale):
    bsz, L, _ = u.shape
    uf = u.astype(jnp.float32)
    s0 = jnp.concatenate([jnp.zeros_like(uf[:, :1]), jnp.cumsum(uf, axis=1)], axis=1)
    t = jnp.arange(L)
    outs = []
    for k, w in enumerate(POOL_WINDOWS):
        lo, hi = k * POOL_GROUP, (k + 1) * POOL_GROUP
        sk = s0[:, :, lo:hi]
        lag = jnp.concatenate([jnp.zeros((bsz, w - 1, POOL_GROUP), jnp.float32), sk[:, :L - w + 1]], axis=1)
        cnt = jnp.minimum(t + 1, w).astype(jnp.float32)[None, :, None]
        d = ((sk[:, 1:] - lag) / cnt - uf[:, :, lo:hi]).astype(u.dtype)
        outs.append(d @ w_pool[k])
    return jnp.concatenate(outs, axis=-1) * pool_scale


def causal_dwconv(x, w, b):
    K = w.shape[0]
    L = x.shape[1]
    xp = jnp.pad(x, ((0, 0), (K - 1, 0), (0, 0)))
    y = xp[:, 0:L] * w[0]
    for k in range(1, K):
        y = y + xp[:, k:k + L] * w[k]
    return y + b


def ssd_chunked(xh, dt, A, bm, cm):
    bsz, lp = xh.shape[:2]
    nc = lp // SSD_CHUNK
    X = (xh * dt[..., None]).reshape(bsz, nc, SSD_CHUNK, SSD_GROUPS, SSD_HPG, SSD_HEAD_DIM)
    adt = (dt * A).reshape(bsz, nc, SSD_CHUNK, SSD_GROUPS, SSD_HPG)
    bc = bm.reshape(bsz, nc, SSD_CHUNK, SSD_GROUPS, SSD_STATE)
    cc = cm.reshape(bsz, nc, SSD_CHUNK, SSD_GROUPS, SSD_STATE)
    a_cs = jnp.cumsum(adt, axis=2)
    causal = jnp.tril(jnp.ones((SSD_CHUNK, SSD_CHUNK), dtype=bool))[None, None, :, :, None, None]
    diff = a_cs[:, :, :, None] - a_cs[:, :, None, :]
    cb = jnp.einsum('bclgn,bcsgn->bclsg', cc, bc)
    scores = cb[..., None] * jnp.exp(jnp.where(causal, diff, -jnp.inf))
    y_diag = jnp.einsum('bclsgr,bcsgrp->bclgrp', scores, X)
    decay_states = jnp.exp(a_cs[:, :, -1:] - a_cs)
    states = jnp.einsum('bclgn,bclgrp->bcgrpn', bc, X * decay_states[..., None])
    chunk_decay = jnp.exp(a_cs[:, :, -1])

    def step(h, inp):
        st, dec = inp
        return h * dec[..., None, None] + st, h

    h0 = jnp.zeros_like(states[:, 0])
    _, prev = lax.scan(step, h0, (jnp.moveaxis(states, 1, 0), jnp.moveaxis(chunk_decay, 1, 0)))
    prev = jnp.moveaxis(prev, 0, 1)
    y_off = jnp.einsum('bclgn,bcgrpn->bclgrp', cc, prev) * jnp.exp(a_cs)[..., None]
    return (y_diag + y_off).reshape(bsz, lp, SSD_HEADS, SSD_HEAD_DIM)


def ssd_mixer(z, xbc, dt_raw, conv_w, conv_b, dt_bias, a_log, d_skip, ssd_norm):
    bsz, L, _ = z.shape
    xbc = jax.nn.silu(causal_dwconv(xbc, conv_w, conv_b)).astype(jnp.float32)
    xs = xbc[..., :SSD_INNER].reshape(bsz, L, SSD_HEADS, SSD_HEAD_DIM)
    bm = xbc[..., SSD_INNER:SSD_INNER + SSD_GROUPS * SSD_STATE].reshape(bsz, L, SSD_GROUPS, SSD_STATE)
    cm = xbc[..., SSD_INNER + SSD_GROUPS * SSD_STATE:].reshape(bsz, L, SSD_GROUPS, SSD_STATE)
    dt = jax.nn.softplus(dt_raw.astype(jnp.float32) + dt_bias.astype(jnp.float32))
    A = -jnp.exp(a_log.astype(jnp.float32))
    padf = lambda a: jnp.pad(a, ((0, 0), (SSD_LEAD_PAD, 0)) + ((0, 0),) * (a.ndim - 2))
    y = ssd_chunked(padf(xs), padf(dt), A, padf(bm), padf(cm))[:, SSD_LEAD_PAD:]
    y = y + d_skip.astype(jnp.float32)[:, None] * xs
    y = y.reshape(bsz, L, SSD_INNER) * jax.nn.silu(z.astype(jnp.float32))
    yg = y.reshape(bsz, L, SSD_GROUPS, SSD_INNER // SSD_GROUPS)
    yg = yg * lax.rsqrt(jnp.mean(yg * yg, axis=-1, keepdims=True) + EPS)
    return (yg.reshape(bsz, L, SSD_INNER) * ssd_norm.astype(jnp.float32)).astype(z.dtype)


def hier_moe(h, w_rg, b_rg, w_re, b_re, w_gu, w_dn):
    bsz, L, D = h.shape
    T = bsz * L
    hf = h.reshape(T, D)
    g_prob = jax.nn.softmax((hf @ w_rg).astype(jnp.float32) + b_rg.astype(jnp.float32), axis=-1)
    g_p, g_idx = lax.top_k(g_prob, 1)
    e_logits = ((hf @ w_re).astype(jnp.float32) + b_re.astype(jnp.float32)).reshape(T, N_EXPERT_GROUPS, EXPERTS_PER_GROUP)
    e_logits = jnp.take_along_axis(e_logits, g_idx[:, :, None], axis=1)[:, 0]
    e_p, e_idx = lax.top_k(jax.nn.softmax(e_logits, axis=-1), TOP_K)
    e_p = e_p / jnp.sum(e_p, axis=-1, keepdims=True)
    weights = g_p * e_p
    experts = g_idx * EXPERTS_PER_GROUP + e_idx
    A = T * TOP_K
    flat_e = experts.reshape(A).astype(jnp.int32)
    flat_w = weights.reshape(A)
    order = jnp.argsort(flat_e)
    se = flat_e[order]
    stok = (order // TOP_K).astype(jnp.int32)
    sw = flat_w[order]
    counts = jnp.bincount(flat_e, length=N_EXPERTS).astype(jnp.int32)
    starts = jnp.cumsum(counts) - counts
    pcounts = (counts + MOE_BLOCK - 1) // MOE_BLOCK * MOE_BLOCK
    pends = jnp.cumsum(pcounts)
    pstarts = pends - pcounts
    dest = pstarts[se] + (jnp.arange(A, dtype=jnp.int32) - starts[se])
    nb = -(-A // MOE_BLOCK) + N_EXPERTS
    P = nb * MOE_BLOCK
    row_tok = jnp.full((P,), T, jnp.int32).at[dest].set(stok)
    row_w = jnp.zeros((P,), jnp.float32).at[dest].set(sw)
    block_e = jnp.minimum(jnp.searchsorted(pends, jnp.arange(nb, dtype=jnp.int32) * MOE_BLOCK, side='right'), N_EXPERTS - 1)
    h_pad = jnp.concatenate([hf, jnp.zeros((1, D), hf.dtype)], axis=0)
    xb = h_pad[row_tok].reshape(nb, MOE_BLOCK, D)

    def expert_block(args):
        xblk, e = args
        gu = xblk @ w_gu[e]
        g, u = jnp.split(gu, 2, axis=-1)
        return (jax.nn.silu(g) * u) @ w_dn[e]

    yb = lax.map(expert_block, (xb, block_e)).reshape(P, D)
    y = yb * row_w[:, None].astype(yb.dtype)
    out = jnp.zeros((T + 1, D), yb.dtype).at[row_tok].add(y)[:T]
    return out.reshape(bsz, L, D)


def setup_inputs(seed: int = 0) -> dict:
    key = jax.random.key(seed)
    ks = jax.random.split(key, 24)
    f = jnp.float32
    nrm = lambda k, shape, s: jax.random.normal(k, shape, f) * s
    dt0 = jnp.exp(jax.random.uniform(ks[8], (DEPTH, SSD_HEADS), f) * (np.log(0.1) - np.log(0.001)) + np.log(0.001))
    return {
        'x': nrm(ks[0], (BATCH, SEQ, D_MODEL), 1.0),
        'meta_tokens': nrm(ks[1], (N_META, D_MODEL), 1.0),
        'norm_mix': 1.0 + nrm(ks[2], (DEPTH, D_MODEL), 0.05),
        'w_in': nrm(ks[3], (DEPTH, D_MODEL, D_IN_PROJ), D_MODEL ** -0.5),
        'pool_w': nrm(ks[4], (DEPTH, len(POOL_WINDOWS), POOL_GROUP, POOL_GROUP), POOL_GROUP ** -0.5),
        'pool_scale': 1.0 + nrm(ks[5], (DEPTH, POOL_WIDTH), 0.05),
        'conv_w': nrm(ks[6], (DEPTH, SSD_CONV, SSD_CONV_DIM), SSD_CONV ** -0.5),
        'conv_b': nrm(ks[7], (DEPTH, SSD_CONV_DIM), 0.02),
        'dt_bias': dt0 + jnp.log(-jnp.expm1(-dt0)),
        'a_log': jnp.log(jax.random.uniform(ks[9], (DEPTH, SSD_HEADS), f, 1.0, 16.0)),
        'd_skip': 1.0 + nrm(ks[10], (DEPTH, SSD_HEADS), 0.1),
        'ssd_norm': 1.0 + nrm(ks[11], (DEPTH, SSD_INNER), 0.05),
        'w_pool_out': nrm(ks[12], (DEPTH, POOL_WIDTH, D_MODEL), POOL_WIDTH ** -0.5),
        'w_ssd_out': nrm(ks[13], (DEPTH, SSD_INNER, D_MODEL), SSD_INNER ** -0.5),
        'w_out': nrm(ks[14], (DEPTH, D_MODEL, D_MODEL), D_MODEL ** -0.5),
        'norm_ffn': 1.0 + nrm(ks[15], (DEPTH, D_MODEL), 0.05),
        'w_router_group': nrm(ks[16], (DEPTH, D_MODEL, N_EXPERT_GROUPS), D_MODEL ** -0.5),
        'b_router_group': nrm(ks[17], (DEPTH, N_EXPERT_GROUPS), 0.01),
        'w_router_expert': nrm(ks[18], (DEPTH, D_MODEL, N_EXPERTS), D_MODEL ** -0.5),
        'b_router_expert': nrm(ks[19], (DEPTH, N_EXPERTS), 0.01),
        'w_gate_up': nrm(ks[20], (DEPTH, N_EXPERTS, D_MODEL, 2 * D_EXPERT), D_MODEL ** -0.5),
        'w_down': nrm(ks[21], (DEPTH, N_EXPERTS, D_EXPERT, D_MODEL), D_EXPERT ** -0.5),
        'norm_final': 1.0 + nrm(ks[22], (D_MODEL,), 0.05),
    }


def reference(x, meta_tokens, norm_mix, w_in, pool_w, pool_scale, conv_w, conv_b, dt_bias, a_log, d_skip,
              ssd_norm, w_pool_out, w_ssd_out, w_out, norm_ffn, w_router_group, b_router_group,
              w_router_expert, b_router_expert, w_gate_up, w_down, norm_final):
    bsz = x.shape[0]
    meta = jnp.broadcast_to(meta_tokens.astype(x.dtype)[None], (bsz, N_META, D_MODEL))
    h = jnp.concatenate([meta, x], axis=1)
    for i in range(DEPTH):
        hn = rms_norm(h, norm_mix[i])
        proj = hn @ w_in[i]
        u, z, xbc, dt_raw, g_pool, g_ssd = jnp.split(proj, IN_SPLITS, axis=-1)
        y_pool = pool_mixer(u, pool_w[i], pool_scale[i]) @ w_pool_out[i]
        y_ssd = ssd_mixer(z, xbc, dt_raw, conv_w[i], conv_b[i], dt_bias[i], a_log[i], d_skip[i], ssd_norm[i]) @ w_ssd_out[i]
        merged = jax.nn.sigmoid(g_pool) * y_pool + jax.nn.sigmoid(g_ssd) * y_ssd
        h = h + merged @ w_out[i]
        h = h + hier_moe(rms_norm(h, norm_ffn[i]), w_router_group[i], b_router_group[i],
                         w_router_expert[i], b_router_expert[i], w_gate_up[i], w_down[i])
    return rms_norm(h, norm_final)[:, N_META:]
```

```python
import numpy as np
from contextlib import ExitStack
import concourse.bass as bass
import concourse.mybir as mybir
from concourse.bass_utils import run_bass_kernel_spmd

F32 = mybir.dt.float32
BF16 = mybir.dt.bfloat16
I32 = mybir.dt.int32
U32 = mybir.dt.uint32
AF = mybir.ActivationFunctionType
ALU = mybir.AluOpType
AX = mybir.AxisListType

D = 1024
EPS = 1e-6
NEXP = 64
DIN = 8224
C_DTB, C_ALOG, C_DSK, C_BIAS72, C_IOTA, C_EC = 0, 32, 64, 96, 168, 232
C_NMIX, C_PSC, C_SNORM, C_NFFN, C_CW, C_CB, C_DTM = 296, 304, 312, 328, 336, 432, 456


class Buf:
    __slots__ = ("name", "w", "r")

    def __init__(self, name):
        self.name = name
        self.w = None
        self.r = {}


class Chan:
    __slots__ = ("sem", "count")

    def __init__(self, sem):
        self.sem = sem
        self.count = 0


class Trk:
    ENG = ("pe", "act", "dve", "pool", "sp")

    def __init__(self, nc, stack):
        self.nc = nc
        self.h = {"pe": nc.tensor, "act": nc.scalar, "dve": nc.vector, "pool": nc.gpsimd, "sp": nc.sync}
        self.sem = {e: stack.enter_context(nc.semaphore("prog_" + e)) for e in self.ENG}
        self.cnt = {e: 0 for e in self.ENG}
        self.seen = {e: {} for e in self.ENG}
        self.stack = stack
        self.nchan = 0
        self.chans = []

    def chan(self):
        self.nchan += 1
        c = Chan(self.stack.enter_context(self.nc.semaphore("ch%d" % self.nchan)))
        self.chans.append(c)
        return c

    def barrier(self):
        for e in self.ENG:
            for e2 in self.ENG:
                if e2 != e and e2 != "sp" and self.cnt[e2] > 0:
                    self._wait(e, ("e", e2, self.cnt[e2]))
            for c in self.chans:
                if c.count > 0:
                    self._wait(e, ("d", c, c.count))

    def _wait(self, eng, tok):
        if tok[0] == "e":
            key = ("e", tok[1])
            sem = self.sem[tok[1]]
        else:
            key = ("d", id(tok[1]))
            sem = tok[1].sem
        if self.seen[eng].get(key, 0) >= tok[2]:
            return
        self.seen[eng][key] = tok[2]
        self.h[eng].wait_ge(sem, tok[2])

    def op(self, eng, fn, reads=(), writes=(), chan=None):
        is_dma = chan is not None
        for b in reads:
            t = b.w
            if t is not None:
                if t[0] == "e" and t[1] == eng and eng == "pe" and not is_dma:
                    continue
                self._wait(eng, t)
        for b in writes:
            t = b.w
            if t is not None and not (t[0] == "e" and t[1] == eng and not is_dma):
                self._wait(eng, t)
            for t in b.r.values():
                if t[0] == "e" and t[1] == eng and not is_dma:
                    continue
                self._wait(eng, t)
        ins = fn(self.h[eng])
        if is_dma:
            chan.count += 16
            ins.then_inc(chan.sem, 16)
            tok = ("d", chan, chan.count)
            rkey = ("d", id(chan))
        else:
            self.cnt[eng] += 1
            ins.then_inc(self.sem[eng], 1)
            tok = ("e", eng, self.cnt[eng])
            rkey = ("e", eng)
        for b in reads:
            b.r[rkey] = tok
        for b in writes:
            b.w = tok
            b.r = {}
        return tok


def build_nc(NPRE, NMAIN, CAP, dbg=False):
    NSLOT = NPRE + NMAIN
    NCON = C_DTM + NPRE
    NROWS = NEXP * CAP
    nc = bass.Bass("TRN2", target_bir_lowering=False)
    dram_in = lambda n, s, d=F32: nc.dram_tensor(n, s, d, kind="ExternalInput").ap()
    xin = dram_in("xin", [NSLOT * 128, D])
    consts_d = dram_in("consts", [128, NCON])
    cmats_d = dram_in("cmats", [128, 5 * 128])
    nfin_d = dram_in("nfin", [128, D])
    pmats_d = dram_in("pmats", [128, 8 * 128])
    w_in_d = dram_in("w_in", [D, DIN])
    pool_w_d = dram_in("pool_w", [1024, 256])
    w_po_d = dram_in("w_pool_out", [1024, 1024])
    w_so_d = dram_in("w_ssd_out", [2048, 1024])
    w_out_d = dram_in("w_out", [1024, 1024])
    w_rt_d = dram_in("w_router", [1024, 72])
    w_gu_d = dram_in("w_gate_up", [NEXP * 1024, 1024])
    w_dn_d = dram_in("w_down", [NEXP * 512, 1024])
    out_d = nc.dram_tensor("out", [NMAIN * 128, D], F32, kind="ExternalOutput").ap()
    ynT_scr = nc.dram_tensor("ynT_scr", [NMAIN, 128, 2048], BF16).ap()
    h1_scr = nc.dram_tensor("h1_scr", [NMAIN, 128, D], F32).ap()
    xe_scr = nc.dram_tensor("xe_scr", [NROWS, D], BF16).ap()
    y_scr = nc.dram_tensor("y_scr", [NROWS, D], F32).ap()
    dbg_d = {}
    if dbg:
        for n, s, d in (("d_ynT", [128, 2048], BF16), ("d_h1", [128, D], F32), ("d_rt", [128, 8], F32)):
            dbg_d[n] = nc.dram_tensor(n, s, d, kind="ExternalOutput").ap()

    with ExitStack() as top:
        T = Trk(nc, top)
        sb = lambda st, n, s, d: st.enter_context(nc.sbuf_tensor("s_" + n, s, d))
        pb = [top.enter_context(nc.psum_tensor("pb%d" % i, [128, 512], F32)) for i in range(8)]
        PB = [Buf("pb%d" % i) for i in range(8)]
        pbf = [p[:].bitcast(BF16) for p in pb]

        consts = sb(top, "consts", [128, NCON], F32); B_consts = Buf("consts")
        cm_f = sb(top, "cm_f", [128, 5, 128], F32); B_cmf = Buf("cm_f")
        cm_b = sb(top, "cm_b", [128, 5, 128], BF16); B_cmb = Buf("cm_b")
        aneg = sb(top, "aneg", [128, 32], F32); B_aneg = Buf("aneg")
        slots_all = sb(top, "slots_all", [128, NMAIN, 2], I32); B_slots = Buf("slots")
        wts_all = sb(top, "wts_all", [128, NMAIN, 2], F32); B_wts = Buf("wts")
        cast_i = [0]
        ring = {}

        def make_ring(st, tag, n, width=1024):
            ring["n"] = n
            ring["w"] = width
            ring["stg"] = [sb(st, "stg%s%d" % (tag, i), [128, width], F32) for i in range(n)]
            ring["B"] = [Buf("stg%d" % i) for i in range(n)]
            ring["ch"] = [T.chan() for _ in range(n)]
            ring["i"] = 0

        def ring_next():
            i = ring["i"] % ring["n"]
            ring["i"] += 1
            return i, ring["stg"][i], ring["B"][i], ring["ch"][i]
        IDENT, TRIU, LK, ONES, USTR = 0, 1, 2, 3, 4
        ch_misc = T.chan()
        T.op("sp", lambda e: e.dma_start(out=consts[:], in_=consts_d[:, :]), writes=[B_consts], chan=ch_misc)
        ch_misc2 = T.chan()
        T.op("sp", lambda e: e.dma_start(out=cm_f[:].rearrange("p a b -> p (a b)"), in_=cmats_d[:, :]),
             writes=[B_cmf], chan=ch_misc2)
        T.op("dve", lambda e: e.tensor_copy(out=cm_b[:], in_=cm_f[:]), reads=[B_cmf], writes=[B_cmb])
        T.op("act", lambda e: e.activation(out=aneg[:], in_=consts[:, C_ALOG:C_ALOG + 32], func=AF.Exp),
             reads=[B_consts], writes=[B_aneg])
        T.op("dve", lambda e: e.tensor_scalar(out=aneg[:], in0=aneg[:], scalar1=-1.0, scalar2=None, op0=ALU.mult),
             reads=[B_aneg], writes=[B_aneg])
        T.op("dve", lambda e: e.memset(wts_all[:], 0.0), writes=[B_wts])
        bound_reg = nc.gpsimd.to_reg(NROWS - 1)

        def cast_op(dst, src, scale, reads, writes):
            k = cast_i[0] % 3
            cast_i[0] += 1
            if k == 0:
                if scale is None:
                    T.op("act", lambda e: e.copy(out=dst, in_=src), reads=reads, writes=writes)
                else:
                    T.op("act", lambda e: e.activation(out=dst, in_=src, func=AF.Copy, scale=scale),
                         reads=reads + [B_consts], writes=writes)
            elif k == 1:
                if scale is None:
                    T.op("dve", lambda e: e.tensor_copy(out=dst, in_=src), reads=reads, writes=writes)
                else:
                    T.op("dve", lambda e: e.tensor_scalar(out=dst, in0=src, scalar1=scale, scalar2=None, op0=ALU.mult),
                         reads=reads + [B_consts], writes=writes)
            else:
                if scale is None:
                    T.op("pool", lambda e: e.tensor_copy(out=dst, in_=src), reads=reads, writes=writes)
                else:
                    T.op("pool", lambda e: e.tensor_scalar(out=dst, in0=src, scalar1=scale, scalar2=1.0,
                                                           op0=ALU.mult, op1=ALU.mult),
                         reads=reads + [B_consts], writes=writes)

        def load_w(dst, B_dst, src, row0, nk, col0, ncol, scale_col=None, dcol0=0):
            for kc in range(nk):
                c = 0
                while c < ncol:
                    w = min(ring["w"], ncol - c)
                    i, stg_t, B_st, ch_st = ring_next()
                    r0 = row0 + kc * 128
                    T.op("sp", lambda e: e.dma_start(
                        out=stg_t[:, 0:w], in_=src[r0:r0 + 128, col0 + c:col0 + c + w]),
                        writes=[B_st], chan=ch_st)
                    sc = None if scale_col is None else consts[:, scale_col + kc:scale_col + kc + 1]
                    cast_op(dst[:, kc, dcol0 + c:dcol0 + c + w], stg_t[:, 0:w], sc, [B_st], [B_dst])
                    c += w

        def front(st_name, st):
            x_t = sb(st, "x_t" + st_name, [128, D], F32)
            fr = dict(x_t=x_t, B_x=Buf("x_t"), ch_x=T.chan(),
                      ss=sb(st, "ss" + st_name, [128, 4], F32), B_ss=Buf("ss"),
                      xbf=sb(st, "xbf" + st_name, [128, D], BF16), B_xbf=Buf("xbf"),
                      xT=sb(st, "xT" + st_name, [128, 8, 128], BF16), B_xT=Buf("xT"))
            fr["junk"] = fr["xbf"]
            fr["B_junk"] = fr["B_xbf"]
            return fr

        def front_load(fr, slot):
            T.op("sp", lambda e: e.dma_start(out=fr["x_t"][:], in_=xin[slot * 128:(slot + 1) * 128, :]),
                 writes=[fr["B_x"]], chan=fr["ch_x"])

        def rms_rstd(src, B_src, ss, B_ss, junk, B_junk, n):
            T.op("act", lambda e: e.activation(out=junk, in_=src, func=AF.Square, accum_out=ss[:, 0:1]),
                 reads=[B_src], writes=[B_junk, B_ss])
            T.op("act", lambda e: e.activation(out=ss[:, 1:2], in_=ss[:, 0:1], func=AF.Sqrt, scale=1.0 / n, bias=EPS),
                 reads=[B_ss], writes=[B_ss])
            T.op("dve", lambda e: e.reciprocal(out=ss[:, 2:3], in_=ss[:, 1:2]), reads=[B_ss], writes=[B_ss])

        def front_compute(fr):
            front_pre(fr)
            front_tr(fr)

        def front_pre(fr):
            x_t, ss = fr["x_t"], fr["ss"]
            rms_rstd(x_t[:], fr["B_x"], ss, fr["B_ss"], fr["junk"][:], fr["B_junk"], D)
            T.op("pool", lambda e: e.tensor_scalar(out=fr["xbf"][:], in0=x_t[:], scalar1=ss[:, 2:3], scalar2=1.0,
                                                   op0=ALU.mult, op1=ALU.mult),
                 reads=[fr["B_x"], fr["B_ss"]], writes=[fr["B_xbf"]])

        def front_tr(fr):

            def tr(e):
                for k in range(8):
                    i = e.transpose(out=pbf[0][:, k * 128:(k + 1) * 128], in_=fr["xbf"][:, k * 128:(k + 1) * 128],
                                    identity=cm_b[:, IDENT, :])
                return i
            T.op("pe", tr, reads=[fr["B_xbf"], B_cmb], writes=[PB[0]])
            T.op("dve", lambda e: e.tensor_copy(out=fr["xT"][:].rearrange("p a b -> p (a b)"), in_=pbf[0][:, :]),
                 reads=[PB[0]], writes=[fr["B_xT"]])

        with ExitStack() as sa:
            wA = sb(sa, "wA", [128, 8, 5152], BF16); B_wA = Buf("wA")
            dg = sb(sa, "dg", [128, 24, 4, 128], BF16); B_dg = Buf("dg")
            tmpA = ExitStack()
            make_ring(tmpA, "A", 8, 2048)
            load_w(wA, B_wA, w_in_d, 0, 8, 1024, 5152, scale_col=C_NMIX)
            for m in range(24):
                for k in range(4):
                    col = C_CW + m * 4 + k
                    eng = "dve" if (m * 4 + k) % 2 == 0 else "pool"
                    if eng == "dve":
                        T.op("dve", lambda e, m=m, k=k, col=col: e.tensor_scalar(
                            out=dg[:, m, k, :], in0=cm_f[:, IDENT, :], scalar1=consts[:, col:col + 1], scalar2=None,
                            op0=ALU.mult), reads=[B_cmf, B_consts], writes=[B_dg])
                    else:
                        T.op("pool", lambda e, m=m, k=k, col=col: e.tensor_scalar(
                            out=dg[:, m, k, :], in0=cm_f[:, IDENT, :], scalar1=consts[:, col:col + 1], scalar2=1.0,
                            op0=ALU.mult, op1=ALU.mult), reads=[B_cmf, B_consts], writes=[B_dg])
            tmpA.close()
            T.barrier()
            fr = front("A", sa)
            xT2 = [fr["xT"], sb(sa, "xTA1", [128, 8, 128], BF16)]
            B_xT2 = [fr["B_xT"], Buf("xT1")]
            xbcb = sb(sa, "xbcb", [128, 24, 131], BF16); B_xbcb = Buf("xbcb")
            xsT = sb(sa, "xsT", [128, 24, 128], BF16); B_xsT = Buf("xsT")
            Xb = sb(sa, "Xb", [128, 2048], BF16); B_X = Buf("X")
            xsd = sb(sa, "xsd", [128, 2048], BF16); B_xsd = Buf("xsd")
            Xd = sb(sa, "Xd", [128, 2048], BF16); B_Xd = Buf("Xd")
            Btok = sb(sa, "Btok", [128, 512], BF16); B_Btok = Buf("Btok")
            Rm2 = [sb(sa, "Rm%d" % i, [128, 8, 128], F32) for i in range(2)]; B_R2 = [Buf("R0"), Buf("R1")]
            LT2 = [sb(sa, "LT%d" % i, [128, 1024], BF16) for i in range(2)]; B_LT2 = [Buf("LT0"), Buf("LT1")]
            sc2 = [sb(sa, "sc%d" % i, [128, 8, 128], BF16) for i in range(2)]; B_sc2 = [Buf("sc0"), Buf("sc1")]
            t12 = [sb(sa, "t1%d" % i, [128, 512], F32) for i in range(2)]; B_t12 = [Buf("t10"), Buf("t11")]
            cbm = sb(sa, "cbm", [128, 4, 128], BF16); B_cbm = Buf("cbm")
            sz = sb(sa, "sz", [128, 2048], F32); B_sz = Buf("sz")
            yn = sb(sa, "yn", [128, 2048], BF16); B_yn = Buf("yn")
            ynT = sb(sa, "ynT", [128, 2048], BF16); B_ynT = Buf("ynT"); ch_ynT = T.chan()
            S = sb(sa, "S", [128, 4, 512], F32); B_S = Buf("S")
            S_bf = sb(sa, "S_bf", [128, 4, 512], BF16); B_Sbf = Buf("S_bf")
            sm2 = [sb(sa, "sm%d" % i, [128, 12, 32], F32) for i in range(2)]; B_sm2 = [Buf("sm0"), Buf("sm1")]
            ssg = sb(sa, "ssg", [128, 12], F32); B_ssg = Buf("ssg")
            DTR, AX_, EX, LN, DT, ADT, ACS, TOT, DD, DS, EACS, CD = range(12)
            B_scr = [Buf("ynT_scr%d" % c) for c in range(NMAIN)]

            T.op("dve", lambda e: e.memset(xbcb[:], 0.0), writes=[B_xbcb])
            T.op("dve", lambda e: e.memset(S[:], 0.0), writes=[B_S])
            T.op("pool", lambda e: e.memset(S_bf[:], 0.0), writes=[B_Sbf])

            def is_main(slot):
                return slot >= NPRE

            def F_front(slot):
                F_front_pre(slot)
                F_front_tr(slot)

            def F_front_pre(slot):
                front_pre(fr)
                if slot + 1 < NSLOT:
                    front_load(fr, slot + 1)

            def F_front_tr(slot):
                par = slot % 2
                fr["xT"], fr["B_xT"] = xT2[par], B_xT2[par]
                front_tr(fr)

            def F_ngroups(slot):
                return 6 if (is_main(slot) or slot == NPRE - 1) else 5

            def F_inproj_group(slot, gi, banks):
                par = slot % 2
                xT, B_xT = xT2[par], B_xT2[par]
                m0 = gi * 4
                bank = banks[gi % 2]

                def mmf(e):
                    for j in range(4):
                        m = m0 + j
                        for kc in range(8):
                            i = e.matmul(pb[bank][:, j * 128:(j + 1) * 128],
                                         lhsT=wA[:, kc, 2048 + m * 128:2048 + (m + 1) * 128], rhs=xT[:, kc, :],
                                         start=(kc == 0), stop=(kc == 7))
                    return i
                T.op("pe", mmf, reads=[B_wA, B_xT], writes=[PB[bank]])
                if gi % 2 == 0:
                    T.op("act", lambda e: e.copy(
                        out=xbcb[:, m0:m0 + 4, 3:131], in_=pb[bank][:].rearrange("p (a b) -> p a b", a=4)),
                        reads=[PB[bank]], writes=[B_xbcb])
                else:
                    T.op("dve", lambda e: e.tensor_copy(
                        out=xbcb[:, m0:m0 + 4, 3:131], in_=pb[bank][:].rearrange("p (a b) -> p a b", a=4)),
                        reads=[PB[bank]], writes=[B_xbcb])
                dripA(2)

            chainA = []

            def CH(eng, fn, reads=(), writes=(), chan=None):
                chainA.append(lambda: T.op(eng, fn, reads=reads, writes=writes, chan=chan))

            def dripA(n):
                for _ in range(n):
                    if chainA:
                        chainA.pop(0)()

            def F_dt(slot, dbank):
                par = slot % 2
                xT, B_xT = xT2[par], B_xT2[par]
                sm, B_sm = sm2[par], B_sm2[par]

                def mmdt(e):
                    for kc in range(8):
                        i = e.matmul(pb[dbank][:, 0:32], lhsT=xT[:, kc, :], rhs=wA[:, kc, 5120:5152],
                                     start=(kc == 0), stop=(kc == 7))
                    return i
                CH("pe", mmdt, reads=[B_wA, B_xT], writes=[PB[dbank]])
                CH("dve", lambda e: e.tensor_tensor(out=sm[:, DTR, :], in0=pb[dbank][:, 0:32], in1=consts[:, C_DTB:C_DTB + 32],
                                                      op=ALU.add), reads=[PB[dbank], B_consts], writes=[B_sm])
                CH("act", lambda e: e.activation(out=sm[:, AX_, :], in_=sm[:, DTR, :], func=AF.Abs),
                     reads=[B_sm], writes=[B_sm])
                CH("act", lambda e: e.activation(out=sm[:, EX, :], in_=sm[:, AX_, :], func=AF.Exp, scale=-1.0),
                     reads=[B_sm], writes=[B_sm])
                CH("act", lambda e: e.activation(out=sm[:, LN, :], in_=sm[:, EX, :], func=AF.Ln, bias=1.0),
                     reads=[B_sm], writes=[B_sm])
                CH("dve", lambda e: e.scalar_tensor_tensor(out=sm[:, DT, :], in0=sm[:, DTR, :], scalar=0.0, in1=sm[:, LN, :],
                                                             op0=ALU.max, op1=ALU.add), reads=[B_sm], writes=[B_sm])
                if not is_main(slot):
                    CH("dve", lambda e: e.tensor_scalar(
                        out=sm[:, DT, :], in0=sm[:, DT, :], scalar1=consts[:, C_DTM + slot:C_DTM + slot + 1], scalar2=None,
                        op0=ALU.mult), reads=[B_sm, B_consts], writes=[B_sm])
                CH("dve", lambda e: e.tensor_tensor(out=sm[:, ADT, :], in0=sm[:, DT, :], in1=aneg[:], op=ALU.mult),
                     reads=[B_sm, B_aneg], writes=[B_sm])

            def F_dt2(slot, dbank):
                par = slot % 2
                sm, B_sm = sm2[par], B_sm2[par]

                def mmcs(e):
                    e.matmul(pb[dbank][:, 32:64], lhsT=cm_f[:, TRIU, :], rhs=sm[:, ADT, :], start=True, stop=True)
                    return e.matmul(pb[dbank][:, 64:96], lhsT=cm_f[:, ONES, :], rhs=sm[:, ADT, :], start=True, stop=True)
                CH("pe", mmcs, reads=[B_cmf, B_sm], writes=[PB[dbank]])
                CH("dve", lambda e: e.tensor_copy(out=sm[:, ACS:ACS + 2, :].rearrange("p a b -> p (a b)"), in_=pb[dbank][:, 32:96]),
                     reads=[PB[dbank]], writes=[B_sm])
                CH("dve", lambda e: e.tensor_tensor(out=sm[:, DD, :], in0=sm[:, TOT, :], in1=sm[:, ACS, :], op=ALU.subtract),
                     reads=[B_sm], writes=[B_sm])
                CH("act", lambda e: e.activation(out=sm[:, DS, :], in_=sm[:, DD, :], func=AF.Exp), reads=[B_sm], writes=[B_sm])
                CH("act", lambda e: e.activation(out=sm[:, EACS, :], in_=sm[:, ACS, :], func=AF.Exp), reads=[B_sm], writes=[B_sm])
                CH("act", lambda e: e.activation(out=sm[:, CD, :], in_=sm[:, TOT, :], func=AF.Exp), reads=[B_sm], writes=[B_sm])

            front_load(fr, 0)
            F_front(0)
            for gi in range(F_ngroups(0)):
                F_inproj_group(0, gi, (6, 7))
            F_dt(0, 3)
            F_dt2(0, 3)
            dripA(1000)
            deferred = []

            for slot in range(NSLOT):
                main = is_main(slot)
                cidx = slot - NPRE
                par = slot % 2
                xT, B_xT = xT2[par], B_xT2[par]
                sm, B_sm = sm2[par], B_sm2[par]
                nxt = slot + 1 if slot + 1 < NSLOT else None
                nm = 24 if main else 20
                if nxt is not None:
                    F_front_pre(nxt)
                for gi, m0 in enumerate(range(0, nm, 4)):
                    bank = 6 + (gi % 2)

                    def mmc(e, m0=m0, bank=bank):
                        for j in range(4):
                            m = m0 + j
                            for k in range(4):
                                i = e.matmul(pb[bank][:, j * 128:(j + 1) * 128], lhsT=dg[:, m, k, :], rhs=xbcb[:, m, k:k + 128],
                                             start=(k == 0), stop=(k == 3))
                        return i
                    T.op("pe", mmc, reads=[B_dg, B_xbcb], writes=[PB[bank]])
                    for j in range(4):
                        m = m0 + j
                        T.op("act", lambda e, m=m, j=j, bank=bank: e.activation(
                            out=xsT[:, m, :], in_=pb[bank][:, j * 128:(j + 1) * 128], func=AF.Silu,
                            bias=consts[:, C_CB + m:C_CB + m + 1]), reads=[PB[bank], B_consts], writes=[B_xsT])
                while deferred:
                    deferred.pop(0)()
                if main:
                    def emit_R(g):
                        gp = g % 2
                        reng = "dve" if g % 2 == 0 else "pool"
                        T.op(reng, lambda e: e.tensor_tensor(
                            out=Rm2[gp][:], in0=cm_f[:, TRIU:TRIU + 1, :].to_broadcast([128, 8, 128]),
                            in1=sm[:, ADT, g * 8:(g + 1) * 8].unsqueeze(2).to_broadcast([128, 8, 128]), op=ALU.mult),
                            reads=[B_cmf, B_sm], writes=[B_R2[gp]])

                    def emit_diff(g):
                        gp = g % 2
                        Rm = Rm2[gp]

                        def mmd(e):
                            e.matmul(pb[4][:, :], lhsT=cm_f[:, LK, :], rhs=Rm[:, 0:4, :].rearrange("p a b -> p (a b)"),
                                     start=True, stop=True)
                            return e.matmul(pb[5][:, :], lhsT=cm_f[:, LK, :], rhs=Rm[:, 4:8, :].rearrange("p a b -> p (a b)"),
                                            start=True, stop=True)
                        T.op("pe", mmd, reads=[B_cmf, B_R2[gp]], writes=[PB[4], PB[5]])


                    def emit_exp(g):
                        LT, B_LT = LT2[g % 2], B_LT2[g % 2]
                        for q in range(2):
                            T.op("act", lambda e, q=q: e.activation(out=LT[:, q * 512:(q + 1) * 512], in_=pb[4 + q][:, :],
                                                                    func=AF.Exp), reads=[PB[4 + q]], writes=[B_LT])

                    emit_R(0)
                    emit_diff(0)
                    emit_R(1)
                    emit_exp(0)
                    emit_diff(1)
                if main:
                    for n in range(4):
                        bank = 2 + (n % 2)

                        def mmz(e, n=n, bank=bank):
                            for kc in range(8):
                                i = e.matmul(pb[bank][:, :], lhsT=xT[:, kc, :], rhs=wA[:, kc, n * 512:(n + 1) * 512],
                                             start=(kc == 0), stop=(kc == 7))
                            return i
                        T.op("pe", mmz, reads=[B_wA, B_xT], writes=[PB[bank]])
                        T.op("act", lambda e, n=n, bank=bank: e.activation(
                            out=sz[:, n * 512:(n + 1) * 512], in_=pb[bank][:, :], func=AF.Silu),
                            reads=[PB[bank]], writes=[B_sz])
                if nxt is not None:
                    F_front_tr(nxt)
                    F_dt(nxt, 3)
                    F_dt2(nxt, 3)
                    dripA(2)
                    fgroups = list(range(F_ngroups(nxt)))
                else:
                    fgroups = []
                T.op("pool", lambda e: e.tensor_copy(out=xbcb[:, :, 0:3], in_=xbcb[:, :, 128:131]),
                     reads=[B_xbcb], writes=[B_xbcb])

                def trB(e):
                    for g in range(4):
                        i = e.transpose(out=pbf[0][:, g * 128:(g + 1) * 128], in_=xsT[:, 16 + g, :], identity=cm_b[:, IDENT, :])
                    return i
                T.op("pe", trB, reads=[B_xsT, B_cmb], writes=[PB[0]])
                T.op("act", lambda e: e.copy(out=Btok[:], in_=pbf[0][:, 0:512]), reads=[PB[0]], writes=[B_Btok])

                def trX(e):
                    for m in range(16):
                        i = e.transpose(out=pbf[1 + m // 8][:, (m % 8) * 128:(m % 8 + 1) * 128], in_=xsT[:, m, :],
                                        identity=cm_b[:, IDENT, :])
                    return i
                T.op("pe", trX, reads=[B_xsT, B_cmb], writes=[PB[1], PB[2]])
                for hf in range(2):
                    T.op("dve", lambda e, hf=hf: e.tensor_tensor(
                        out=Xb[:, hf * 1024:(hf + 1) * 1024].rearrange("p (h j) -> p h j", h=16),
                        in0=pbf[1 + hf][:, :].rearrange("p (h j) -> p h j", h=16),
                        in1=sm[:, DT, hf * 16:(hf + 1) * 16].unsqueeze(2).to_broadcast([128, 16, 64]), op=ALU.mult),
                        reads=[PB[1 + hf], B_sm], writes=[B_X])
                    if main:
                        T.op("dve", lambda e, hf=hf: e.tensor_tensor(
                            out=xsd[:, hf * 1024:(hf + 1) * 1024].rearrange("p (h j) -> p h j", h=16),
                            in0=pbf[1 + hf][:, :].rearrange("p (h j) -> p h j", h=16),
                            in1=consts[:, C_DSK + hf * 16:C_DSK + (hf + 1) * 16].unsqueeze(2).to_broadcast([128, 16, 64]),
                            op=ALU.mult), reads=[PB[1 + hf], B_consts], writes=[B_xsd])
                T.op("pool", lambda e: e.tensor_tensor(
                    out=Xd[:].rearrange("p (h j) -> p h j", h=32), in0=Xb[:].rearrange("p (h j) -> p h j", h=32),
                    in1=sm[:, DS, :].unsqueeze(2).to_broadcast([128, 32, 64]), op=ALU.mult),
                    reads=[B_X, B_sm], writes=[B_Xd])
                if main:
                    def mmcb(e):
                        for g in range(4):
                            i = e.matmul(pb[3][:, g * 128:(g + 1) * 128], lhsT=xsT[:, 16 + g, :], rhs=xsT[:, 20 + g, :],
                                         start=True, stop=True)
                        return i
                    T.op("pe", mmcb, reads=[B_xsT], writes=[PB[3]])
                    T.op("dve", lambda e: e.tensor_tensor(
                        out=cbm[:], in0=pb[3][:, :].rearrange("p (g l) -> p g l", g=4),
                        in1=cm_f[:, TRIU:TRIU + 1, :].to_broadcast([128, 4, 128]), op=ALU.mult),
                        reads=[PB[3], B_cmf], writes=[B_cbm])
                if main:
                    for g in range(4):
                        gp = g % 2
                        LT, B_LT, sc, B_sc, t1, B_t1 = LT2[gp], B_LT2[gp], sc2[gp], B_sc2[gp], t12[gp], B_t12[gp]
                        if g + 2 < 4:
                            emit_R(g + 2)
                        for _ in range(2):
                            if fgroups:
                                F_inproj_group(nxt, fgroups.pop(0), (6, 7))
                        if g >= 1:
                            emit_exp(g)
                            if g + 1 < 4:
                                emit_diff(g + 1)
                        T.op("dve", lambda e, g=g, sc=sc, LT=LT: e.tensor_tensor(
                            out=sc[:], in0=LT[:].rearrange("p (h l) -> p h l", h=8),
                            in1=cbm[:, g:g + 1, :].to_broadcast([128, 8, 128]), op=ALU.mult),
                            reads=[B_LT, B_cbm], writes=[B_sc])
                        dripA(1)

                        def mmy(e, g=g, sc=sc):
                            e.matmul(pb[0][:, :], lhsT=cm_b[:, IDENT, :], rhs=xsd[:, g * 512:(g + 1) * 512], start=True, stop=False)
                            for h in range(8):
                                i = e.matmul(pb[0][:, h * 64:(h + 1) * 64], lhsT=sc[:, h, :],
                                             rhs=Xb[:, (g * 8 + h) * 64:(g * 8 + h + 1) * 64], start=False, stop=(h == 7))
                            return i
                        T.op("pe", mmy, reads=[B_cmb, B_xsd, B_sc, B_X], writes=[PB[0]])
                        T.op("pe", lambda e, g=g: e.matmul(pb[1][:, :], lhsT=xsT[:, 20 + g, :], rhs=S_bf[:, g, :], start=True, stop=True),
                             reads=[B_xsT, B_Sbf], writes=[PB[1]])
                        T.op("dve", lambda e, g=g, t1=t1: e.tensor_tensor(
                            out=t1[:].rearrange("p (h j) -> p h j", h=8), in0=pb[1][:, :].rearrange("p (h j) -> p h j", h=8),
                            in1=sm[:, EACS, g * 8:(g + 1) * 8].unsqueeze(2).to_broadcast([128, 8, 64]), op=ALU.mult),
                            reads=[PB[1], B_sm], writes=[B_t1])
                        dripA(1)
                        T.op("dve", lambda e, t1=t1: e.tensor_tensor(out=t1[:], in0=pb[0][:, :], in1=t1[:], op=ALU.add),
                             reads=[PB[0], B_t1], writes=[B_t1])
                        T.op("pool", lambda e, g=g, t1=t1: e.tensor_tensor(out=sz[:, g * 512:(g + 1) * 512], in0=t1[:],
                                                                           in1=sz[:, g * 512:(g + 1) * 512], op=ALU.mult),
                             reads=[B_t1, B_sz], writes=[B_sz])
                        dripA(1)
                        T.op("act", lambda e, g=g: e.activation(out=yn[:, g * 512:(g + 1) * 512], in_=sz[:, g * 512:(g + 1) * 512],
                                                                func=AF.Square, accum_out=ssg[:, g:g + 1]),
                             reads=[B_sz], writes=[B_yn, B_ssg])
                while fgroups:
                    F_inproj_group(nxt, fgroups.pop(0), (6, 7))
                dripA(1000)
                for g in range(4):
                    bank = 2 + (g % 2)
                    T.op("pe", lambda e, g=g, bank=bank: e.matmul(pb[bank][:, :], lhsT=Btok[:, g * 128:(g + 1) * 128],
                                                                  rhs=Xd[:, g * 512:(g + 1) * 512], start=True, stop=True),
                         reads=[B_Btok, B_Xd], writes=[PB[bank]])
                    T.op("pool", lambda e, g=g: e.tensor_tensor(
                        out=S[:, g, :].rearrange("p (h j) -> p h j", h=8), in0=S[:, g, :].rearrange("p (h j) -> p h j", h=8),
                        in1=sm[:, CD, g * 8:(g + 1) * 8].unsqueeze(2).to_broadcast([128, 8, 64]), op=ALU.mult),
                        reads=[B_S, B_sm], writes=[B_S])
                    T.op("dve", lambda e, g=g, bank=bank: e.tensor_tensor(out=S[:, g, :], in0=pb[bank][:, :], in1=S[:, g, :], op=ALU.add),
                         reads=[PB[bank], B_S], writes=[B_S])
                T.op("pool", lambda e: e.tensor_copy(out=S_bf[:], in_=S[:]), reads=[B_S], writes=[B_Sbf])
                if main:
                    T.op("act", lambda e: e.activation(out=ssg[:, 4:8], in_=ssg[:, 0:4], func=AF.Sqrt, scale=1.0 / 512, bias=EPS),
                         reads=[B_ssg], writes=[B_ssg])
                    T.op("dve", lambda e: e.reciprocal(out=ssg[:, 8:12], in_=ssg[:, 4:8]), reads=[B_ssg], writes=[B_ssg])
                    for g in range(4):
                        T.op("act", lambda e, g=g: e.activation(out=yn[:, g * 512:(g + 1) * 512], in_=sz[:, g * 512:(g + 1) * 512],
                                                                func=AF.Copy, scale=ssg[:, 8 + g:9 + g]),
                             reads=[B_sz, B_ssg], writes=[B_yn])

                    def tail(cidx=cidx):
                        def trY(e):
                            for m in range(16):
                                i = e.transpose(out=pbf[4 + m // 8][:, (m % 8) * 128:(m % 8 + 1) * 128],
                                                in_=yn[:, m * 128:(m + 1) * 128], identity=cm_b[:, IDENT, :])
                            return i
                        T.op("pe", trY, reads=[B_yn, B_cmb], writes=[PB[4], PB[5]])
                        T.op("dve", lambda e: e.tensor_copy(out=ynT[:, 0:1024], in_=pbf[4][:, :]), reads=[PB[4]], writes=[B_ynT])
                        T.op("act", lambda e: e.copy(out=ynT[:, 1024:2048], in_=pbf[5][:, :]), reads=[PB[5]], writes=[B_ynT])
                        T.op("sp", lambda e: e.dma_start(out=ynT_scr[cidx, :, :], in_=ynT[:]),
                             reads=[B_ynT], writes=[B_scr[cidx]], chan=ch_ynT)
                        if dbg and cidx == 0:
                            chd = T.chan()
                            T.op("sp", lambda e: e.dma_start(out=dbg_d["d_ynT"][:, :], in_=ynT[:]), reads=[B_ynT], chan=chd)
                    deferred.append(tail)
            while deferred:
                deferred.pop(0)()

        T.barrier()
        B_h1s = [Buf("h1_scr%d" % c) for c in range(NMAIN)]
        B_xe = Buf("xe_scr")
        with ExitStack() as sbk:
            wB = sb(sbk, "wB", [128, 8, 3072], BF16); B_wB = Buf("wB")
            pw = sb(sbk, "pw", [128, 8, 256], BF16); B_pw = Buf("pw")
            wpo = sb(sbk, "wpo", [128, 8, 1024], BF16); B_wpo = Buf("wpo")
            wso = sb(sbk, "wso", [128, 16, 1024], BF16); B_wso = Buf("wso")
            wou = sb(sbk, "wou", [128, 8, 1024], BF16); B_wou = Buf("wou")
            wrt = sb(sbk, "wrt", [128, 8, 72], BF16); B_wrt = Buf("wrt")
            pm_b = sb(sbk, "pm_b", [128, 8, 128], BF16); B_pmb = Buf("pm_b")
            tmpB = ExitStack()
            make_ring(tmpB, "B", 8, 2048)
            load_w(wB, B_wB, w_in_d, 0, 8, 0, 1024, scale_col=C_NMIX, dcol0=0)
            load_w(wB, B_wB, w_in_d, 0, 8, 6176, 2048, scale_col=C_NMIX, dcol0=1024)
            load_w(pw, B_pw, pool_w_d, 0, 8, 0, 256)
            load_w(wpo, B_wpo, w_po_d, 0, 8, 0, 1024, scale_col=C_PSC)
            load_w(wso, B_wso, w_so_d, 0, 16, 0, 1024, scale_col=C_SNORM)
            load_w(wou, B_wou, w_out_d, 0, 8, 0, 1024)
            load_w(wrt, B_wrt, w_rt_d, 0, 8, 0, 72, scale_col=C_NFFN)
            i, stg_t, B_st, ch_st = ring_next()
            T.op("sp", lambda e: e.dma_start(out=stg_t[:, 0:1024], in_=pmats_d[:, :]), writes=[B_st], chan=ch_st)
            T.op("dve", lambda e: e.tensor_copy(out=pm_b[:].rearrange("p a b -> p (a b)"), in_=stg_t[:, 0:1024]),
                 reads=[B_st], writes=[B_pmb])

            tmpB.close()
            T.barrier()
            fr = front("B", sbk)
            utok = [sb(sbk, "utok%d" % i, [128, D], BF16) for i in range(2)]
            B_utok = [Buf("utok0"), Buf("utok1")]
            sg = sb(sbk, "sg", [128, 2048], F32); B_sg = Buf("sg")
            dT = sb(sbk, "dT", [128, 8, 128], BF16); B_dT = Buf("dT")
            pmT = sb(sbk, "pmT", [128, 8, 128], BF16); B_pmT = Buf("pmT")
            ynTl = sb(sbk, "ynTl", [128, 16, 128], BF16); B_ynTl = Buf("ynTl"); ch_ynTl = T.chan()
            m1 = sb(sbk, "m1", [128, D], F32); B_m1 = Buf("m1")
            m2 = sb(sbk, "m2", [128, D], F32); B_m2 = Buf("m2")
            mg = sb(sbk, "mg", [128, D], BF16); B_mg = Buf("mg")
            mT = sb(sbk, "mT", [128, 8, 128], BF16); B_mT = Buf("mT")
            h1 = sb(sbk, "h1", [128, D], F32); B_h1 = Buf("h1"); ch_h1 = T.chan()
            ss2 = sb(sbk, "ss2", [128, 4], F32); B_ss2 = Buf("ss2")
            hn2 = [sb(sbk, "hn%d" % i, [128, D], BF16) for i in range(2)]; B_hn2 = [Buf("hn0"), Buf("hn1")]; ch_sc = T.chan()
            hnT = sb(sbk, "hnT", [128, 8, 128], BF16); B_hnT = Buf("hnT")
            lg = sb(sbk, "lg", [128, 72], F32); B_lg = Buf("lg")
            rt = sb(sbk, "rt", [128, 64], F32); B_rt = Buf("rt")
            top8 = sb(sbk, "top8", [128, 16], F32); B_top8 = Buf("top8")
            idx8 = sb(sbk, "idx8", [128, 8], U32); B_idx8 = Buf("idx8")
            msk = sb(sbk, "msk", [128, 64], F32); B_msk = Buf("msk")
            A1 = sb(sbk, "A1", [128, 3, 64], F32); B_A = Buf("A")
            Ab = sb(sbk, "Ab", [128, 64], BF16); B_Ab = Buf("Ab")
            cntb = sb(sbk, "cntb", [128, 64], F32); B_cnt = Buf("cnt")
            slm = sb(sbk, "slm", [128, 3, 64], F32); B_slm = Buf("slm")
            slf = sb(sbk, "slf", [128, 4], F32); B_slf = Buf("slf")
            ec1 = sb(sbk, "ec1", [128, 64], F32); B_ec1 = Buf("ec1")
            T.op("dve", lambda e: e.tensor_copy(out=cntb[:], in_=consts[:, C_EC:C_EC + 64]), reads=[B_consts], writes=[B_cnt])
            T.op("dve", lambda e: e.tensor_scalar(out=ec1[:], in0=consts[:, C_EC:C_EC + 64], scalar1=float(CAP), scalar2=None,
                                                  op0=ALU.add), reads=[B_consts], writes=[B_ec1])

            frs = [fr, dict(fr)]
            frs[1]["x_t"] = sb(sbk, "x_tB1", [128, D], F32)
            frs[1]["B_x"] = Buf("x_t1")
            frs[1]["ch_x"] = T.chan()
            frs[1]["xT"] = sb(sbk, "xTB1", [128, 8, 128], BF16)
            frs[1]["B_xT"] = Buf("xT1")

            def load_ynT(ci):
                T.op("sp", lambda e: e.dma_start(out=ynTl[:].rearrange("p a b -> p (a b)"), in_=ynT_scr[ci, :, :]),
                     reads=[B_scr[ci]], writes=[B_ynTl], chan=ch_ynTl)

            def FB(slot):
                par = slot % 2
                f = frs[par]
                xT = f["xT"]
                front_compute(f)
                for n in range(2):
                    bank = 1 + n

                    def mmu(e, n=n, bank=bank):
                        for kc in range(8):
                            i = e.matmul(pb[bank][:, :], lhsT=xT[:, kc, :], rhs=wB[:, kc, n * 512:(n + 1) * 512],
                                         start=(kc == 0), stop=(kc == 7))
                        return i
                    T.op("pe", mmu, reads=[B_wB, f["B_xT"]], writes=[PB[bank]])
                    T.op("act", lambda e, n=n, bank=bank: e.copy(out=utok[par][:, n * 512:(n + 1) * 512], in_=pb[bank][:, :]),
                         reads=[PB[bank]], writes=[B_utok[par]])
                if slot < NPRE:
                    return
                for n in range(4):
                    bank = 3 + (n % 2)

                    def mmg(e, n=n, bank=bank):
                        for kc in range(8):
                            i = e.matmul(pb[bank][:, :], lhsT=xT[:, kc, :], rhs=wB[:, kc, 1024 + n * 512:1024 + (n + 1) * 512],
                                         start=(kc == 0), stop=(kc == 7))
                        return i
                    T.op("pe", mmg, reads=[B_wB, f["B_xT"]], writes=[PB[bank]])
                    T.op("act", lambda e, n=n, bank=bank: e.activation(out=sg[:, n * 512:(n + 1) * 512], in_=pb[bank][:, :],
                                                                       func=AF.Sigmoid), reads=[PB[bank]], writes=[B_sg])

            front_load(frs[(NPRE - 1) % 2], NPRE - 1)
            front_load(frs[NPRE % 2], NPRE)
            FB(NPRE - 1)
            if NPRE + 1 < NSLOT:
                front_load(frs[(NPRE + 1) % 2], NPRE + 1)
            FB(NPRE)
            load_ynT(0)
            deferredB = []

            def drip(n):
                for _ in range(n):
                    if deferredB:
                        deferredB.pop(0)()
            def XA(slot):
                cidx = slot - NPRE
                par = slot % 2
                for half in range(2):
                    bank = 5 + half

                    def mmw(e, half=half, bank=bank):
                        for j in range(4):
                            fc = half * 4 + j
                            k = fc // 2
                            e.matmul(pb[bank][:, j * 128:(j + 1) * 128], lhsT=utok[1 - par][:, fc * 128:(fc + 1) * 128],
                                     rhs=pm_b[:, 2 * k + 1, :], start=True, stop=False)
                            i = e.matmul(pb[bank][:, j * 128:(j + 1) * 128], lhsT=utok[par][:, fc * 128:(fc + 1) * 128],
                                         rhs=pm_b[:, 2 * k, :], start=False, stop=True)
                        return i
                    T.op("pe", mmw, reads=[B_utok[0], B_utok[1], B_pmb], writes=[PB[bank]])
                    T.op("act", lambda e, half=half, bank=bank: e.copy(
                        out=dT[:, half * 4:(half + 1) * 4, :].rearrange("p a b -> p (a b)"), in_=pb[bank][:, :]),
                        reads=[PB[bank]], writes=[B_dT])
                    drip(2)
                for half in range(2):
                    bank = 5 + half

                    def mmp(e, half=half, bank=bank):
                        for j in range(4):
                            oc = half * 4 + j
                            k = oc // 2
                            o2 = oc % 2
                            for kc in range(2):
                                i = e.matmul(pb[bank][:, j * 128:(j + 1) * 128], lhsT=pw[:, k * 2 + kc, o2 * 128:(o2 + 1) * 128],
                                             rhs=dT[:, 2 * k + kc, :], start=(kc == 0), stop=(kc == 1))
                        return i
                    T.op("pe", mmp, reads=[B_pw, B_dT], writes=[PB[bank]])
                    T.op("act", lambda e, half=half, bank=bank: e.copy(
                        out=pmT[:, half * 4:(half + 1) * 4, :].rearrange("p a b -> p (a b)"), in_=pb[bank][:, :]),
                        reads=[PB[bank]], writes=[B_pmT])
                    drip(2)

            def XB(slot):
                cidx = slot - NPRE
                par = slot % 2
                for n in range(2):
                    bank = 1 + n

                    def mmyp(e, n=n, bank=bank):
                        for kc in range(8):
                            i = e.matmul(pb[bank][:, :], lhsT=pmT[:, kc, :], rhs=wpo[:, kc, n * 512:(n + 1) * 512],
                                         start=(kc == 0), stop=(kc == 7))
                        return i
                    T.op("pe", mmyp, reads=[B_pmT, B_wpo], writes=[PB[bank]])
                    T.op("dve", lambda e, n=n, bank=bank: e.tensor_tensor(out=m1[:, n * 512:(n + 1) * 512], in0=pb[bank][:, :],
                                                                          in1=sg[:, n * 512:(n + 1) * 512], op=ALU.mult),
                         reads=[PB[bank], B_sg], writes=[B_m1])
                    drip(2)
                for n in range(2):
                    bank = 3 + n

                    def mmys(e, n=n, bank=bank):
                        for kc in range(16):
                            i = e.matmul(pb[bank][:, :], lhsT=ynTl[:, kc, :], rhs=wso[:, kc, n * 512:(n + 1) * 512],
                                         start=(kc == 0), stop=(kc == 15))
                        return i
                    T.op("pe", mmys, reads=[B_ynTl, B_wso], writes=[PB[bank]])
                    T.op("dve", lambda e, n=n, bank=bank: e.tensor_tensor(out=m2[:, n * 512:(n + 1) * 512], in0=pb[bank][:, :],
                                                                          in1=sg[:, 1024 + n * 512:1024 + (n + 1) * 512], op=ALU.mult),
                         reads=[PB[bank], B_sg], writes=[B_m2])
                    drip(2)
                if cidx + 1 < NMAIN:
                    load_ynT(cidx + 1)

            def Y1(slot):
                cidx = slot - NPRE
                par = slot % 2
                drip(4)
                T.op("pool", lambda e: e.tensor_tensor(out=mg[:], in0=m1[:], in1=m2[:], op=ALU.add),
                     reads=[B_m1, B_m2], writes=[B_mg])

                def trM(e):
                    for k in range(8):
                        i = e.transpose(out=pbf[0][:, k * 128:(k + 1) * 128], in_=mg[:, k * 128:(k + 1) * 128], identity=cm_b[:, IDENT, :])
                    return i
                T.op("pe", trM, reads=[B_mg, B_cmb], writes=[PB[0]])
                T.op("act", lambda e: e.copy(out=mT[:].rearrange("p a b -> p (a b)"), in_=pbf[0][:, :]), reads=[PB[0]], writes=[B_mT])
                drip(2)

            def Y2(slot):
                cidx = slot - NPRE
                par = slot % 2
                for n in range(2):
                    bank = 5 + n

                    def mmo(e, n=n, bank=bank):
                        for kc in range(8):
                            i = e.matmul(pb[bank][:, :], lhsT=mT[:, kc, :], rhs=wou[:, kc, n * 512:(n + 1) * 512],
                                         start=(kc == 0), stop=(kc == 7))
                        return i
                    T.op("pe", mmo, reads=[B_mT, B_wou], writes=[PB[bank]])
                    T.op("dve", lambda e, n=n, bank=bank: e.tensor_tensor(out=h1[:, n * 512:(n + 1) * 512], in0=pb[bank][:, :],
                                                                          in1=frs[par]["x_t"][:, n * 512:(n + 1) * 512], op=ALU.add),
                         reads=[PB[bank], frs[par]["B_x"]], writes=[B_h1])
                    drip(2)
                T.op("sp", lambda e, cidx=cidx: e.dma_start(out=h1_scr[cidx, :, :], in_=h1[:]),
                     reads=[B_h1], writes=[B_h1s[cidx]], chan=ch_h1)
                if dbg and cidx == 0:
                    chd = T.chan()
                    T.op("sp", lambda e: e.dma_start(out=dbg_d["d_h1"][:, :], in_=h1[:]), reads=[B_h1], chan=chd)
                rms_rstd(h1[:], B_h1, ss2, B_ss2, m1[:].bitcast(BF16)[:, 0:D], B_m1, D)
                hn, B_hn = hn2[par], B_hn2[par]
                T.op("act", lambda e: e.activation(out=hn[:], in_=h1[:], func=AF.Copy, scale=ss2[:, 2:3]),
                     reads=[B_h1, B_ss2], writes=[B_hn])


            def Y3(slot):
                cidx = slot - NPRE
                par = slot % 2
                hn, B_hn = hn2[par], B_hn2[par]
                drip(1000)

                def trH(e):
                    for k in range(8):
                        i = e.transpose(out=pbf[0][:, k * 128:(k + 1) * 128], in_=hn[:, k * 128:(k + 1) * 128], identity=cm_b[:, IDENT, :])
                    return i
                T.op("pe", trH, reads=[B_hn, B_cmb], writes=[PB[0]])
                T.op("act", lambda e: e.copy(out=hnT[:].rearrange("p a b -> p (a b)"), in_=pbf[0][:, :]),
                     reads=[PB[0]], writes=[B_hnT])

                def mmr(e):
                    for kc in range(8):
                        i = e.matmul(pb[7][:, 0:72], lhsT=hnT[:, kc, :], rhs=wrt[:, kc, :], start=(kc == 0), stop=(kc == 7))
                    return i
                T.op("pe", mmr, reads=[B_hnT, B_wrt], writes=[PB[7]])
                T.op("dve", lambda e: e.tensor_tensor(out=lg[:], in0=pb[7][:, 0:72], in1=consts[:, C_BIAS72:C_BIAS72 + 72], op=ALU.add),
                     reads=[PB[7], B_consts], writes=[B_lg])
                chain = []
                hn_c, B_hn_c = hn2[par], B_hn2[par]

                def ch(eng, fn, reads=(), writes=(), chan=None):
                    chain.append(lambda: T.op(eng, fn, reads=reads, writes=writes, chan=chan))

                ch("dve", lambda e: e.max(out=top8[:, 0:8], in_=lg[:, 0:8]), reads=[B_lg], writes=[B_top8])
                ch("dve", lambda e: e.tensor_scalar(out=rt[:, 1:2], in0=top8[:, 0:1], scalar1=-1.0, scalar2=None, op0=ALU.mult),
                   reads=[B_top8], writes=[B_rt])
                ch("dve", lambda e: e.tensor_scalar(out=rt[:, 24:32], in0=lg[:, 0:8], scalar1=top8[:, 0:1], scalar2=-1e30,
                                                    op0=ALU.is_lt, op1=ALU.mult), reads=[B_lg, B_top8], writes=[B_rt])
                ch("act", lambda e: e.activation(out=rt[:, 8:16], in_=lg[:, 0:8], func=AF.Exp, bias=rt[:, 1:2], accum_out=rt[:, 2:3]),
                   reads=[B_lg, B_rt], writes=[B_rt])
                ch("dve", lambda e: e.tensor_tensor(out=msk[:].rearrange("p (g j) -> p g j", g=8),
                                                    in0=lg[:, 8:72].rearrange("p (g j) -> p g j", g=8),
                                                    in1=rt[:, 24:32].unsqueeze(2).to_broadcast([128, 8, 8]), op=ALU.add),
                   reads=[B_lg, B_rt], writes=[B_msk])
                ch("dve", lambda e: e.max(out=top8[:, 8:16], in_=msk[:]), reads=[B_msk], writes=[B_top8])
                ch("dve", lambda e: e.reciprocal(out=rt[:, 3:4], in_=rt[:, 2:3]), reads=[B_rt], writes=[B_rt])
                ch("dve", lambda e: e.tensor_tensor(out=rt[:, 4:5], in0=top8[:, 8:9], in1=top8[:, 9:10], op=ALU.subtract),
                   reads=[B_top8], writes=[B_rt])
                for j in range(2):
                    ch("dve", lambda e, j=j: e.tensor_scalar(out=A1[:, j, :], in0=msk[:], scalar1=top8[:, 8 + j:9 + j],
                                                             scalar2=None, op0=ALU.is_equal), reads=[B_msk, B_top8], writes=[B_A])
                ch("act", lambda e: e.activation(out=rt[:, 5:6], in_=rt[:, 4:5], func=AF.Sigmoid), reads=[B_rt], writes=[B_rt])
                ch("act", lambda e: e.activation(out=rt[:, 6:7], in_=rt[:, 4:5], func=AF.Sigmoid, scale=-1.0), reads=[B_rt], writes=[B_rt])
                ch("dve", lambda e: e.tensor_tensor(out=Ab[:], in0=A1[:, 0, :], in1=A1[:, 1, :], op=ALU.add), reads=[B_A], writes=[B_Ab])

                def mmpos(e):
                    e.matmul(pb[7][:, 128:192], lhsT=cm_b[:, USTR, :], rhs=Ab[:], start=True, stop=True)
                    return e.matmul(pb[7][:, 256:320], lhsT=cm_b[:, ONES, :], rhs=Ab[:], start=True, stop=True)
                ch("pe", mmpos, reads=[B_cmb, B_Ab], writes=[PB[7]])
                ch("dve", lambda e, cidx=cidx: e.tensor_scalar(out=wts_all[:, cidx, :], in0=rt[:, 5:7], scalar1=rt[:, 3:4], scalar2=None,
                                                               op0=ALU.mult), reads=[B_rt], writes=[B_wts])
                ch("dve", lambda e: e.tensor_tensor(out=slm[:, 0, :], in0=pb[7][:, 128:192], in1=cntb[:], op=ALU.add),
                   reads=[PB[7], B_cnt], writes=[B_slm])
                ch("dve", lambda e: e.tensor_tensor(out=cntb[:], in0=pb[7][:, 256:320], in1=cntb[:], op=ALU.add),
                   reads=[PB[7], B_cnt], writes=[B_cnt])
                ch("dve", lambda e: e.tensor_tensor(out=slm[:, 1, :], in0=slm[:, 0, :], in1=ec1[:], op=ALU.is_ge),
                   reads=[B_slm, B_ec1], writes=[B_slm])
                ch("dve", lambda e: e.scalar_tensor_tensor(out=slm[:, 2, :], in0=slm[:, 1, :], scalar=1e6, in1=slm[:, 0, :],
                                                           op0=ALU.mult, op1=ALU.add), reads=[B_slm], writes=[B_slm])
                for j in range(2):
                    ch("dve", lambda e, j=j: e.tensor_tensor(out=A1[:, j, :], in0=A1[:, j, :], in1=slm[:, 2, :], op=ALU.mult),
                       reads=[B_A, B_slm], writes=[B_A])
                ch("dve", lambda e: e.reduce_sum(out=slf[:, 2:4], in_=A1[:, 0:2, :], axis=AX.X), reads=[B_A], writes=[B_slf])
                ch("dve", lambda e, cidx=cidx: e.tensor_copy(out=slots_all[:, cidx, :], in_=slf[:, 2:4]), reads=[B_slf], writes=[B_slots])
                if dbg and cidx == 0:
                    chd = T.chan()
                    ch("dve", lambda e: e.tensor_copy(out=rt[:, 40:42], in_=slf[:, 2:4]), reads=[B_slf], writes=[B_rt])
                    ch("dve", lambda e: e.tensor_copy(out=rt[:, 42:44], in_=wts_all[:, 0, :]), reads=[B_wts], writes=[B_rt])
                    ch("sp", lambda e: e.dma_start(out=dbg_d["d_rt"][:, :], in_=rt[:, 40:48]), reads=[B_rt], chan=chd)
                for j in range(2):
                    ch("pool", lambda e, cidx=cidx, j=j, hn_c=hn_c: e.indirect_dma_start(
                        out=xe_scr[:, :], out_offset=bass.IndirectOffsetOnAxis(ap=slots_all[:, cidx, j:j + 1], axis=0),
                        in_=hn_c[:], in_offset=None, bounds_check=bound_reg, oob_is_err=False),
                        reads=[B_hn_c, B_slots], writes=[B_xe], chan=ch_sc)
                deferredB.extend(chain)

            XA(NPRE)
            XB(NPRE)
            if NPRE + 1 < NSLOT:
                FB(NPRE + 1)
            for slot in range(NPRE, NSLOT):
                Y1(slot)
                if slot + 1 < NSLOT:
                    XA(slot + 1)
                Y2(slot)
                if slot + 2 < NSLOT:
                    front_load(frs[slot % 2], slot + 2)
                if slot + 1 < NSLOT:
                    XB(slot + 1)
                Y3(slot)
                if slot + 2 < NSLOT:
                    FB(slot + 2)
            while deferredB:
                deferredB.pop(0)()

        T.barrier()
        B_ys = Buf("y_scr")
        with ExitStack() as s2:
            wgu = [sb(s2, "wgu%d" % i, [128, 8, 1024], BF16) for i in range(2)]
            B_wgu = [Buf("wgu0"), Buf("wgu1")]
            wdn = [sb(s2, "wdn%d" % i, [128, 4, 1024], BF16) for i in range(2)]
            B_wdn = [Buf("wdn0"), Buf("wdn1")]
            NT = CAP // 128
            xe = [[sb(s2, "xe%d_%d" % (q, i), [128, D], BF16) for i in range(NT)] for q in range(2)]
            B_xet = [[Buf("xe%d_%d" % (q, i)) for i in range(NT)] for q in range(2)]
            ch_xe = [[T.chan() for _ in range(NT)] for q in range(2)]

            def load_xe(ex):
                q = ex % 2
                for t in range(NT):
                    T.op("sp", lambda e, t=t: e.dma_start(out=xe[q][t][:], in_=xe_scr[ex * CAP + t * 128:ex * CAP + (t + 1) * 128, :]),
                         reads=[B_xe], writes=[B_xet[q][t]], chan=ch_xe[q][t])
            xeT = sb(s2, "xeT", [128, 8, CAP], BF16); B_xeT = Buf("xeT")
            gs = sb(s2, "gs", [128, CAP], F32); B_gs = Buf("gs")
            actT = sb(s2, "actT", [128, 4, CAP], BF16); B_actT = Buf("actT")
            ysb = [sb(s2, "ysb%d" % i, [128, D], F32) for i in range(2)]
            B_ysb = [Buf("ysb0"), Buf("ysb1")]
            ch_ys = [T.chan(), T.chan()]
            ysi = [0]
            make_ring(s2, "E", 24)

            def weight_steps(ex):
                p = ex % 2
                steps = []
                for kc in range(8):
                    steps.append((wgu[p], B_wgu[p], w_gu_d, ex * 1024 + kc * 128, kc, C_NFFN + kc))
                for kc in range(4):
                    steps.append((wdn[p], B_wdn[p], w_dn_d, ex * 512 + kc * 128, kc, None))
                return steps

            def issue_dma(step):
                dst, B_dst, src, r0, kc, scol = step
                i, stg_t, B_st, ch_st = ring_next()
                T.op("sp", lambda e: e.dma_start(out=stg_t[:, :], in_=src[r0:r0 + 128, :]), writes=[B_st], chan=ch_st)
                return i

            def issue_cast(step, i):
                dst, B_dst, src, r0, kc, scol = step
                sc_ = None if scol is None else consts[:, scol:scol + 1]
                cast_op(dst[:, kc, :], ring["stg"][i][:, :], sc_, [ring["B"][i]], [B_dst])

            load_xe(0)
            st0 = weight_steps(0)
            ids = [issue_dma(s) for s in st0]
            st1 = weight_steps(1)
            ahead = list(zip(st1, [issue_dma(s) for s in st1]))
            for s, i in zip(st0, ids):
                issue_cast(s, i)
            for ex in range(NEXP):
                p = ex % 2
                if ex + 1 < NEXP:
                    load_xe(ex + 1)
                pend = ahead
                nxt2 = weight_steps(ex + 2) if ex + 2 < NEXP else []
                ahead = list(zip(nxt2, [issue_dma(s) for s in nxt2]))
                for t in range(NT):
                    def trE(e, t=t):
                        for k in range(8):
                            i = e.transpose(out=pbf[t % 2][:, k * 128:(k + 1) * 128], in_=xe[p][t][:, k * 128:(k + 1) * 128],
                                            identity=cm_b[:, IDENT, :])
                        return i
                    T.op("pe", trE, reads=[B_xet[p][t], B_cmb], writes=[PB[t % 2]])
                    T.op("dve", lambda e, t=t: e.tensor_copy(out=xeT[:, :, t * 128:(t + 1) * 128],
                                                             in_=pbf[t % 2][:, :].rearrange("p (a b) -> p a b", a=8)),
                         reads=[PB[t % 2]], writes=[B_xeT])
                for m in range(4):
                    bg = 2 + 2 * (m % 2)
                    bu = bg + 1

                    def mmgu(e, m=m, bg=bg, bu=bu):
                        for kc in range(8):
                            e.matmul(pb[bg][:, 0:CAP], lhsT=wgu[p][:, kc, m * 128:(m + 1) * 128], rhs=xeT[:, kc, :],
                                     start=(kc == 0), stop=(kc == 7))
                        for kc in range(8):
                            i = e.matmul(pb[bu][:, 0:CAP], lhsT=wgu[p][:, kc, 512 + m * 128:512 + (m + 1) * 128], rhs=xeT[:, kc, :],
                                         start=(kc == 0), stop=(kc == 7))
                        return i
                    T.op("pe", mmgu, reads=[B_wgu[p], B_xeT], writes=[PB[bg], PB[bu]])
                    T.op("act", lambda e, bg=bg: e.activation(out=gs[:], in_=pb[bg][:, 0:CAP], func=AF.Silu), reads=[PB[bg]], writes=[B_gs])
                    T.op("dve", lambda e, m=m, bu=bu: e.tensor_tensor(out=actT[:, m, :], in0=pb[bu][:, 0:CAP], in1=gs[:], op=ALU.mult),
                         reads=[PB[bu], B_gs], writes=[B_actT])
                    for _ in range(2):
                        if pend:
                            s, i = pend.pop(0)
                            issue_cast(s, i)
                for t in range(NT):
                    q = ysi[0] % 2
                    ysi[0] += 1
                    for n in range(2):
                        bank = 6 + n

                        def mmdn(e, t=t, n=n, bank=bank):
                            for kc in range(4):
                                i = e.matmul(pb[bank][:, :], lhsT=actT[:, kc, t * 128:(t + 1) * 128], rhs=wdn[p][:, kc, n * 512:(n + 1) * 512],
                                             start=(kc == 0), stop=(kc == 3))
                            return i
                        T.op("pe", mmdn, reads=[B_actT, B_wdn[p]], writes=[PB[bank]])
                        T.op("act", lambda e, n=n, bank=bank, q=q: e.copy(out=ysb[q][:, n * 512:(n + 1) * 512], in_=pb[bank][:, :]),
                             reads=[PB[bank]], writes=[B_ysb[q]])
                    T.op("sp", lambda e, t=t, q=q: e.dma_start(out=y_scr[ex * CAP + t * 128:ex * CAP + (t + 1) * 128, :], in_=ysb[q][:]),
                         reads=[B_ysb[q]], writes=[B_ys], chan=ch_ys[q])
                    for _ in range(2):
                        if pend:
                            s, i = pend.pop(0)
                            issue_cast(s, i)
                while pend:
                    s, i = pend.pop(0)
                    issue_cast(s, i)

        T.barrier()
        with ExitStack() as s3:
            NB3 = 3
            yg_ = [[sb(s3, "yga%d_%d" % (q, i), [128, D], F32) for i in range(2)] for q in range(NB3)]
            B_yga = [[Buf("yga"), Buf("yga")] for q in range(NB3)]
            ch_g = [[T.chan(), T.chan()] for q in range(NB3)]
            h1l = [sb(s3, "h1l%d" % q, [128, D], F32) for q in range(NB3)]
            B_h1l = [Buf("h1l") for q in range(NB3)]
            ch_h1l = [T.chan() for q in range(NB3)]
            h2 = [sb(s3, "h2_%d" % q, [128, D], F32) for q in range(2)]
            B_h2 = [Buf("h2"), Buf("h2")]
            ob = [sb(s3, "ob%d" % q, [128, D], F32) for q in range(2)]
            B_ob = [Buf("ob"), Buf("ob")]
            ch_ob = [T.chan(), T.chan()]
            ss3 = sb(s3, "ss3", [128, 4], F32); B_ss3 = Buf("ss3")
            junk3 = sb(s3, "junk3", [128, D], BF16); B_junk3 = Buf("junk3")
            wv = sb(s3, "wv", [128, 4], F32); B_wv = Buf("wv")
            slf3 = sb(s3, "slf3", [128, 2], F32); B_slf3 = Buf("slf3")
            nfin = sb(s3, "nfin", [128, D], F32); B_nfin = Buf("nfin"); ch_nf = T.chan()
            T.op("sp", lambda e: e.dma_start(out=nfin[:], in_=nfin_d[:, :]), writes=[B_nfin], chan=ch_nf)
            for q in range(NB3):
                for i in range(2):
                    T.op("dve", lambda e, q=q, i=i: e.memset(yg_[q][i][:], 0.0), writes=[B_yga[q][i]])

            def p3_load(c):
                q = c % NB3
                T.op("sp", lambda e: e.dma_start(out=h1l[q][:], in_=h1_scr[c, :, :]), reads=[B_h1s[c]], writes=[B_h1l[q]], chan=ch_h1l[q])
                for j in range(2):
                    T.op("pool", lambda e, j=j: e.indirect_dma_start(
                        out=yg_[q][j][:], out_offset=None, in_=y_scr[:, :],
                        in_offset=bass.IndirectOffsetOnAxis(ap=slots_all[:, c, j:j + 1], axis=0),
                        bounds_check=bound_reg, oob_is_err=False), reads=[B_ys, B_slots], writes=[B_yga[q][j]], chan=ch_g[q][j])

            lasts = []
            for c in range(min(NB3 - 1, NMAIN)):
                p3_load(c)
            for c in range(NMAIN):
                q = c % NB3
                p = c % 2
                if c + NB3 - 1 < NMAIN:
                    p3_load(c + NB3 - 1)
                T.op("dve", lambda e, c=c: e.tensor_copy(out=slf3[:], in_=slots_all[:, c, :]), reads=[B_slots], writes=[B_slf3])
                T.op("dve", lambda e: e.tensor_scalar(out=wv[:, 0:2], in0=slf3[:], scalar1=float(NROWS), scalar2=None, op0=ALU.is_lt),
                     reads=[B_slf3], writes=[B_wv])
                T.op("dve", lambda e, c=c: e.tensor_tensor(out=wv[:, 2:4], in0=wv[:, 0:2], in1=wts_all[:, c, :], op=ALU.mult),
                     reads=[B_wv, B_wts], writes=[B_wv])
                T.op("dve", lambda e: e.scalar_tensor_tensor(out=h2[p][:], in0=yg_[q][0][:], scalar=wv[:, 2:3], in1=h1l[q][:], op0=ALU.mult, op1=ALU.add),
                     reads=[B_yga[q][0], B_wv, B_h1l[q]], writes=[B_h2[p]])
                T.op("dve", lambda e: e.scalar_tensor_tensor(out=h2[p][:], in0=yg_[q][1][:], scalar=wv[:, 3:4], in1=h2[p][:], op0=ALU.mult, op1=ALU.add),
                     reads=[B_yga[q][1], B_wv, B_h2[p]], writes=[B_h2[p]])
                rms_rstd(h2[p][:], B_h2[p], ss3, B_ss3, junk3[:], B_junk3, D)
                T.op("dve", lambda e: e.scalar_tensor_tensor(out=ob[p][:], in0=h2[p][:], scalar=ss3[:, 2:3], in1=nfin[:],
                                                             op0=ALU.mult, op1=ALU.mult), reads=[B_h2[p], B_ss3, B_nfin], writes=[B_ob[p]])
                lasts.append(T.op("sp", lambda e, c=c: e.dma_start(out=out_d[c * 128:(c + 1) * 128, :], in_=ob[p][:]), reads=[B_ob[p]], chan=ch_ob[p]))
            for t in lasts[-2:]:
                T._wait("sp", t)
    return nc


def _const_mats():
    k = np.arange(128)[:, None]
    l = np.arange(128)[None, :]
    cm = np.zeros((128, 5, 128), np.float32)
    cm[:, 0] = (k == l)
    cm[:, 1] = (k <= l)
    cm[:, 2] = (k > l)
    cm[:, 3] = 1.0
    cm[:, 4] = (k < l)
    pm = np.zeros((128, 8, 128), np.float32)
    for i, w in enumerate((2, 4, 8, 16)):
        cur = ((k <= l) & (k > l - w)).astype(np.float32) / w - (k == l)
        prev = ((k - 128) > (l - w)).astype(np.float32) / w
        pm[:, 2 * i] = cur
        pm[:, 2 * i + 1] = prev
    return cm.reshape(128, 640), pm.reshape(128, 1024)


def _pack_consts(inp, dtmask, CAP):
    NPRE = dtmask.shape[1]
    c = np.zeros((128, C_DTM + NPRE), np.float32)
    bc = lambda v: np.broadcast_to(np.asarray(v, np.float32).reshape(1, -1), (128, np.asarray(v).size))
    pp = lambda v: np.asarray(v, np.float32).reshape(-1, 128).T
    c[:, C_DTB:C_DTB + 32] = bc(inp["dt_bias"][0])
    c[:, C_ALOG:C_ALOG + 32] = bc(inp["a_log"][0])
    c[:, C_DSK:C_DSK + 32] = bc(inp["d_skip"][0])
    c[:, C_BIAS72:C_BIAS72 + 8] = bc(inp["b_router_group"][0])
    c[:, C_BIAS72 + 8:C_BIAS72 + 72] = bc(inp["b_router_expert"][0])
    c[:, C_IOTA:C_IOTA + 64] = bc(np.arange(64))
    c[:, C_EC:C_EC + 64] = bc(np.arange(64) * CAP)
    c[:, C_NMIX:C_NMIX + 8] = pp(inp["norm_mix"][0])
    c[:, C_PSC:C_PSC + 8] = pp(inp["pool_scale"][0])
    c[:, C_SNORM:C_SNORM + 16] = pp(inp["ssd_norm"][0])
    c[:, C_NFFN:C_NFFN + 8] = pp(inp["norm_ffn"][0])
    cw = np.asarray(inp["conv_w"][0], np.float32)
    c[:, C_CW:C_CW + 96] = cw.reshape(4, 24, 128).transpose(2, 1, 0).reshape(128, 96)
    c[:, C_CB:C_CB + 24] = pp(inp["conv_b"][0])
    c[:, C_DTM:] = dtmask
    return c


def _shared_maps(inp):
    f = lambda a: np.ascontiguousarray(np.asarray(a, np.float32))
    cm, pm = _const_mats()
    return {
        "cmats": cm, "pmats": pm,
        "nfin": np.ascontiguousarray(np.broadcast_to(f(inp["norm_final"]).reshape(1, D), (128, D))),
        "w_in": f(inp["w_in"][0]),
        "pool_w": f(inp["pool_w"][0]).reshape(1024, 256),
        "w_pool_out": f(inp["w_pool_out"][0]),
        "w_ssd_out": f(inp["w_ssd_out"][0]),
        "w_out": f(inp["w_out"][0]),
        "w_router": np.ascontiguousarray(np.concatenate([f(inp["w_router_group"][0]), f(inp["w_router_expert"][0])], axis=1)),
        "w_gate_up": f(inp["w_gate_up"][0]).reshape(NEXP * 1024, 1024),
        "w_down": f(inp["w_down"][0]).reshape(NEXP * 512, 1024),
    }


CAP_FULL = 256
_NC_CACHE = {}


def kernel(**inputs):
    x = np.asarray(inputs["x"], np.float32)
    meta = np.asarray(inputs["meta_tokens"], np.float32)
    B, L, _ = x.shape
    NPRE, NMAIN = 33, 32
    half_len = NMAIN * 128
    key = (NPRE, NMAIN, CAP_FULL)
    if key not in _NC_CACHE:
        _NC_CACHE[key] = build_nc(NPRE, NMAIN, CAP_FULL)
    nc = _NC_CACHE[key]
    shared = _shared_maps(inputs)
    metachunk = np.zeros((128, D), np.float32)
    metachunk[112:] = meta
    in_maps = []
    for b in range(B):
        for hf in range(2):
            dtm = np.zeros((128, NPRE), np.float32)
            if hf == 0:
                xin = np.concatenate([np.zeros((32 * 128, D), np.float32), metachunk, x[b, :half_len]], axis=0)
                dtm[112:, 32] = 1.0
            else:
                xin = np.concatenate([metachunk, x[b]], axis=0)
                dtm[112:, 0] = 1.0
                dtm[:, 1:] = 1.0
            m = dict(shared)
            m["xin"] = np.ascontiguousarray(xin)
            m["consts"] = _pack_consts(inputs, dtm, CAP_FULL)
            in_maps.append(m)
    res = run_bass_kernel_spmd(nc, in_maps, core_ids=list(range(8)))
    out = np.empty((B, L, D), np.float32)
    for b in range(B):
        for hf in range(2):
            out[b, hf * half_len:(hf + 1) * half_len] = res.results[b * 2 + hf]["out"]
    return out
```
